# Optimizing a Trainium2 kernel written in Bass

```python
import math
import jax, jax.numpy as jnp
from jax import lax
import numpy as np

D_MODEL = 1024
BATCH = 4
SEQ = 8192
DEPTH = 2

CHUNK = 64
N_EVEN = (DEPTH + 1) // 2
N_ODD = DEPTH // 2
EPS = 1e-6
LN_EPS = 1e-5
ROPE_THETA = 10000.0

CONV_DIM = D_MODEL
CONV_WIDTH = 31
RWKV_HEAD = 64
RWKV_HEADS = D_MODEL // RWKV_HEAD
RWKV_DIM = RWKV_HEADS * RWKV_HEAD
LORA_DIM = 64
GN_EPS = 64e-5
ATT_HEADS = 16
ATT_HEAD_DIM = 128
ATT_DIM = ATT_HEADS * ATT_HEAD_DIM
IDX_HEADS = 8
IDX_HEAD_DIM = 64
TOPK_MAX = 256
Q_BLOCK = 128

EVEN_SIZES = (CONV_DIM, CONV_DIM, CONV_DIM,
              RWKV_DIM, RWKV_DIM, RWKV_DIM,
              LORA_DIM, LORA_DIM, RWKV_DIM)
EVEN_COLS = sum(EVEN_SIZES)
EVEN_OUT = CONV_DIM + RWKV_DIM
ODD_SIZES = (ATT_DIM, ATT_HEAD_DIM, ATT_HEAD_DIM,
             IDX_HEADS * IDX_HEAD_DIM, IDX_HEAD_DIM, IDX_HEADS,
             ATT_DIM)
ODD_COLS = sum(ODD_SIZES)

kernel_name = "chunk_causal_hybrid_conv_rwkv7_dsa"


def split_cols(z, sizes):
    idx = np.cumsum(sizes)[:-1].tolist()
    return jnp.split(z, idx, axis=-1)


def rms_norm(x, g):
    xf = x.astype(jnp.float32)
    y = xf * lax.rsqrt(jnp.mean(xf * xf, axis=-1, keepdims=True) + EPS)
    return (y * g.astype(jnp.float32)).astype(x.dtype)


def layer_norm(x, w, b, eps):
    xf = x.astype(jnp.float32)
    mu = jnp.mean(xf, axis=-1, keepdims=True)
    var = jnp.mean(jnp.square(xf - mu), axis=-1, keepdims=True)
    y = (xf - mu) * lax.rsqrt(var + eps)
    return (y * w.astype(jnp.float32) + b.astype(jnp.float32)).astype(x.dtype)


def rope(x, pos):
    d = x.shape[-1]
    inv = ROPE_THETA ** (-jnp.arange(0, d, 2, dtype=jnp.float32) / d)
    ang = pos.astype(jnp.float32)[..., None] * inv
    cos = jnp.cos(ang)[:, :, None, :]
    sin = jnp.sin(ang)[:, :, None, :]
    xf = x.astype(jnp.float32)
    x1, x2 = jnp.split(xf, 2, axis=-1)
    out = jnp.concatenate([x1 * cos - x2 * sin, x2 * cos + x1 * sin], axis=-1)
    return out.astype(x.dtype)


def token_shift(z):
    return jnp.pad(z[:, :-1], ((0, 0), (1, 0), (0, 0)))


def conformer_conv(val, glu, conv_w, conv_vec):
    u = val * jax.nn.sigmoid(glu)
    y = lax.conv_general_dilated(u, conv_w.astype(u.dtype), window_strides=(1,),
                                 padding=[(CONV_WIDTH - 1, 0)],
                                 dimension_numbers=('NWC', 'WIO', 'NWC'),
                                 feature_group_count=CONV_DIM)
    y = y + conv_vec[0]
    y = layer_norm(y, conv_vec[1], conv_vec[2], LN_EPS)
    return jax.nn.silu(y)


def rwkv7_step(state, inp):
    r_t, w_t, k_t, v_t, kk_t, a_t = inp
    sa = jnp.einsum('bhvk,bhk->bhv', state, -kk_t)
    state = (state * w_t[:, :, None, :]
             + sa[..., None] * (kk_t * a_t)[:, :, None, :]
             + v_t[..., None] * k_t[:, :, None, :])
    y = jnp.einsum('bhvk,bhk->bhv', state, r_t)
    return state, y


def rwkv7_mix(r, k, v, wd, ad, mu_rkv, mu_lora, vec, w_up, a_up, r_k):
    B, S, _ = r.shape
    f32 = jnp.float32
    lerp = lambda z, mu: z + (token_shift(z) - z) * mu
    r = lerp(r, mu_rkv[0]).astype(f32)
    k = lerp(k, mu_rkv[1]).astype(f32)
    v = lerp(v, mu_rkv[2]).astype(f32)
    wd = lerp(wd, mu_lora[0]).astype(f32)
    ad = lerp(ad, mu_lora[1]).astype(f32)
    vec = vec.astype(f32)
    w0, a0, k_k, k_a, lnx_w, lnx_b = vec[0], vec[1], vec[2], vec[3], vec[4], vec[5]
    w_log = -jax.nn.softplus(-(w0 + jnp.tanh(wd) @ w_up.astype(f32))) - 0.5
    decay = jnp.exp(-jnp.exp(w_log))
    a = jax.nn.sigmoid(a0 + ad @ a_up.astype(f32))
    heads = lambda t: t.reshape(B, S, RWKV_HEADS, RWKV_HEAD)
    kk = heads(k * k_k)
    kk = kk / jnp.maximum(jnp.sqrt(jnp.sum(kk * kk, axis=-1, keepdims=True)), 1e-12)
    k = k * (1.0 + (a - 1.0) * k_a)
    r_h, w_h, k_h, v_h, a_h = heads(r), heads(decay), heads(k), heads(v), heads(a)
    xs = tuple(jnp.moveaxis(t, 1, 0) for t in (r_h, w_h, k_h, v_h, kk, a_h))
    state0 = jnp.zeros((B, RWKV_HEADS, RWKV_HEAD, RWKV_HEAD), f32)
    _, y = lax.scan(rwkv7_step, state0, xs)
    y = jnp.moveaxis(y, 0, 1)
    mu = jnp.mean(y, axis=-1, keepdims=True)
    var = jnp.mean(jnp.square(y - mu), axis=-1, keepdims=True)
    y = ((y - mu) * lax.rsqrt(var + GN_EPS)).reshape(B, S, RWKV_DIM) * lnx_w + lnx_b
    bonus = jnp.sum(r_h * k_h * r_k.astype(f32), axis=-1, keepdims=True) * v_h
    return y + bonus.reshape(B, S, RWKV_DIM)


def dsa_attention(q, k, v, qi, ki, wi, positions):
    B, S = positions.shape
    k_sel = min(TOPK_MAX, S // 4)
    nb = S // Q_BLOCK
    chunk_id = positions // CHUNK
    kf = ki.astype(jnp.float32)

    def to_blocks(t):
        return jnp.moveaxis(t.reshape((B, nb, Q_BLOCK) + t.shape[2:]), 1, 0)

    def block(args):
        qb, qib, wib, cqb = args
        idx_logits = jnp.einsum('bqhd,bsd->bqhs', qib.astype(jnp.float32), kf) * (IDX_HEAD_DIM ** -0.5)
        score = jnp.einsum('bqh,bqhs->bqs', wib.astype(jnp.float32) * (IDX_HEADS ** -0.5),
                           jax.nn.relu(idx_logits))
        adm = chunk_id[:, None, :] <= cqb[:, :, None]
        score = jnp.where(adm, score, -jnp.inf)
        top_val, top_idx = lax.top_k(score, k_sel)
        valid = jnp.isfinite(top_val)
        kg = jax.vmap(lambda kb, ib: kb[ib])(k, top_idx)
        vg = jax.vmap(lambda vb, ib: vb[ib])(v, top_idx)
        logits = jnp.einsum('bqhd,bqkd->bqhk', qb, kg).astype(jnp.float32) * (ATT_HEAD_DIM ** -0.5)
        logits = jnp.where(valid[:, :, None, :], logits, -jnp.inf)
        p = jax.nn.softmax(logits, axis=-1).astype(v.dtype)
        return jnp.einsum('bqhk,bqkd->bqhd', p, vg)

    o = lax.map(block, (to_blocks(q), to_blocks(qi), to_blocks(wi), to_blocks(chunk_id)))
    return jnp.moveaxis(o, 0, 1).reshape(B, S, ATT_DIM)


def setup_inputs(seed: int = 0) -> dict:
    key = jax.random.key(seed)
    ks = jax.random.split(key, 24)
    nrm = lambda k, shape, s: jax.random.normal(k, shape, jnp.float32) * s
    x = nrm(ks[0], (BATCH, SEQ, D_MODEL), 1.0)
    c = nrm(ks[1], (BATCH, D_MODEL), 1.0)
    offsets = jax.random.randint(ks[2], (BATCH,), 0, 64, dtype=jnp.int32) * CHUNK
    positions = offsets[:, None] + jnp.arange(SEQ, dtype=jnp.int32)[None, :]
    ada_w = nrm(ks[3], (DEPTH, D_MODEL, 3 * D_MODEL), 0.5 * D_MODEL ** -0.5)
    ada_b = nrm(ks[4], (DEPTH, 3 * D_MODEL), 0.02)
    norm_g = 1.0 + nrm(ks[5], (DEPTH, D_MODEL), 0.05)
    final_g = 1.0 + nrm(ks[6], (D_MODEL,), 0.05)
    even_w_in = nrm(ks[7], (N_EVEN, D_MODEL, EVEN_COLS), D_MODEL ** -0.5)
    even_w_out = nrm(ks[8], (N_EVEN, EVEN_OUT, D_MODEL), EVEN_OUT ** -0.5)
    conv_w = nrm(ks[9], (N_EVEN, CONV_WIDTH, 1, CONV_DIM), CONV_WIDTH ** -0.5)
    kc = jax.random.split(ks[10], 3)
    conv_vec = jnp.stack([nrm(kc[0], (N_EVEN, CONV_DIM), 0.02),
                          1.0 + nrm(kc[1], (N_EVEN, CONV_DIM), 0.05),
                          nrm(kc[2], (N_EVEN, CONV_DIM), 0.02)], axis=1)
    rwkv_mu_rkv = jax.random.uniform(ks[11], (N_EVEN, 3, RWKV_DIM), jnp.float32)
    rwkv_mu_lora = jax.random.uniform(ks[12], (N_EVEN, 2, LORA_DIM), jnp.float32)
    kv = jax.random.split(ks[13], 6)
    rwkv_vec = jnp.stack([
        jax.random.uniform(kv[0], (N_EVEN, RWKV_DIM), jnp.float32, -4.0, 1.0),
        nrm(kv[1], (N_EVEN, RWKV_DIM), 0.1),
        0.85 + nrm(kv[2], (N_EVEN, RWKV_DIM), 0.05),
        1.0 + nrm(kv[3], (N_EVEN, RWKV_DIM), 0.05),
        1.0 + nrm(kv[4], (N_EVEN, RWKV_DIM), 0.05),
        nrm(kv[5], (N_EVEN, RWKV_DIM), 0.02)], axis=1)
    rwkv_w_up = nrm(ks[14], (N_EVEN, LORA_DIM, RWKV_DIM), 0.5 * LORA_DIM ** -0.5)
    rwkv_a_up = nrm(ks[15], (N_EVEN, LORA_DIM, RWKV_DIM), 0.5 * LORA_DIM ** -0.5)
    rwkv_r_k = nrm(ks[16], (N_EVEN, RWKV_HEADS, RWKV_HEAD), 0.1)
    odd_w_in = nrm(ks[17], (N_ODD, D_MODEL, ODD_COLS), D_MODEL ** -0.5)
    odd_w_out = nrm(ks[18], (N_ODD, ATT_DIM, D_MODEL), ATT_DIM ** -0.5)
    return {"x": x, "c": c, "positions": positions, "ada_w": ada_w, "ada_b": ada_b,
            "norm_g": norm_g, "final_g": final_g, "even_w_in": even_w_in,
            "even_w_out": even_w_out, "conv_w": conv_w, "conv_vec": conv_vec,
            "rwkv_mu_rkv": rwkv_mu_rkv, "rwkv_mu_lora": rwkv_mu_lora, "rwkv_vec": rwkv_vec,
            "rwkv_w_up": rwkv_w_up, "rwkv_a_up": rwkv_a_up, "rwkv_r_k": rwkv_r_k,
            "odd_w_in": odd_w_in, "odd_w_out": odd_w_out}


def reference(x, c, positions, ada_w, ada_b, norm_g, final_g, even_w_in, even_w_out,
              conv_w, conv_vec, rwkv_mu_rkv, rwkv_mu_lora, rwkv_vec, rwkv_w_up, rwkv_a_up,
              rwkv_r_k, odd_w_in, odd_w_out):
    B, S, _ = x.shape
    cond = jax.nn.silu(c)
    for l in range(DEPTH):
        mod = cond @ ada_w[l] + ada_b[l]
        shift, scale, gate = jnp.split(mod, 3, axis=-1)
        h = rms_norm(x, norm_g[l]) * (1.0 + scale[:, None, :]) + shift[:, None, :]
        j = l // 2
        if l % 2 == 0:
            z = h @ even_w_in[j]
            a_val, a_glu, a_gate, r, k, v, wd, ad, b_gate = split_cols(z, EVEN_SIZES)
            ya = conformer_conv(a_val, a_glu, conv_w[j], conv_vec[j]) * jax.nn.silu(a_gate)
            yb = rwkv7_mix(r, k, v, wd, ad, rwkv_mu_rkv[j], rwkv_mu_lora[j], rwkv_vec[j],
                           rwkv_w_up[j], rwkv_a_up[j], rwkv_r_k[j]).astype(h.dtype)
            yb = yb * jax.nn.silu(b_gate)
            out = jnp.concatenate([ya, yb], axis=-1) @ even_w_out[j]
        else:
            z = h @ odd_w_in[j]
            q, k, v, qi, ki, wi, g = split_cols(z, ODD_SIZES)
            q = rope(q.reshape(B, S, ATT_HEADS, ATT_HEAD_DIM), positions)
            k = rope(k[:, :, None, :], positions)[:, :, 0, :]
            qi = rope(qi.reshape(B, S, IDX_HEADS, IDX_HEAD_DIM), positions)
            ki = rope(ki[:, :, None, :], positions)[:, :, 0, :]
            o = dsa_attention(q, k, v, qi, ki, wi, positions)
            out = (o * jax.nn.silu(g)) @ odd_w_out[j]
        x = x + gate[:, None, :] * out
    return rms_norm(x, final_g)
```

```python
import numpy as np
from contextlib import ExitStack
import concourse.bass as bass
import concourse.mybir as mybir
from concourse.bass_utils import run_bass_kernel_spmd

F32 = mybir.dt.float32
BF16 = mybir.dt.bfloat16
AF = mybir.ActivationFunctionType
ALU = mybir.AluOpType
AX = mybir.AxisListType

S_FULL = 8192
D = 1024
NCORES = 8


class Sched:
    SEM_ROLL = 30000

    def __init__(self, nc, es, ndma=12):
        self.nc = nc
        self.es = es
        self.eng = {'pe': nc.tensor, 'dve': nc.vector, 'act': nc.scalar, 'pool': nc.gpsimd, 'sp': nc.sync}
        self.cur = {}
        self.cnt = {}
        self.nsem = 0
        for e in self.eng:
            self._newsem(e)
        self.dsem = [es.enter_context(nc.semaphore(f"dq{i}")) for i in range(ndma)]
        self.dcnt = [0] * ndma
        self.dnext = 0
        self.seen = {e: {} for e in self.eng}
        self.st = {}
        self.n_inst = 0

    def _newsem(self, e):
        self.cur[e] = self.es.enter_context(self.nc.semaphore(f"s_{e}_{self.nsem}"))
        self.nsem += 1
        self.cnt[e] = 0

    def _state(self, r):
        s = self.st.get(id(r))
        if s is None:
            s = self.st[id(r)] = {'w': None, 'r': [], 'obj': r}
        return s

    def _waits(self, e, reads, writes, extra=()):
        need = {}

        def add(tok):
            if tok is None:
                return
            sem, val, te = tok
            if e == 'pe' and te == 'pe':
                return
            k = id(sem)
            if k not in need or need[k][1] < val:
                need[k] = (sem, val)
        for r in reads:
            add(self._state(r)['w'])
        for w in writes:
            s = self._state(w)
            add(s['w'])
            for t in s['r']:
                add(t)
        for t in extra:
            add(t)
        seen = self.seen[e]
        eng = self.eng[e]
        for k, (sem, val) in need.items():
            if seen.get(k, 0) >= val:
                continue
            eng.wait_ge(sem, val)
            seen[k] = val

    def _commit(self, tok, reads, writes):
        for r in reads:
            self._state(r)['r'].append(tok)
        for w in writes:
            s = self._state(w)
            s['w'] = tok
            s['r'] = []

    def op(self, e, fn, reads=(), writes=()):
        self._waits(e, reads, writes)
        inst = fn(self.eng[e])
        if self.cnt[e] >= self.SEM_ROLL:
            self._newsem(e)
        self.cnt[e] += 1
        inst.then_inc(self.cur[e], 1)
        tok = (self.cur[e], self.cnt[e], e)
        self._commit(tok, reads, writes)
        self.n_inst += 1
        return tok

    def dma(self, q, out, in_, reads=(), writes=(), **kw):
        i = self.dnext
        self.dnext = (self.dnext + 1) % len(self.dsem)
        sem = self.dsem[i]
        prev = (sem, self.dcnt[i], 'dma') if self.dcnt[i] else None
        self._waits(q, reads, writes, extra=(prev,) if prev else ())
        self.dcnt[i] += 16
        self.eng[q].dma_start(out=out, in_=in_, **kw).then_inc(sem, 16)
        tok = (sem, self.dcnt[i], 'dma')
        self._commit(tok, reads, writes)
        self.n_inst += 1
        return tok

    def finish(self, e='sp'):
        toks = []
        for s in self.st.values():
            if s['w']:
                toks.append(s['w'])
            toks.extend(s['r'])
        self._waits(e, (), (), extra=toks)


class Ctx:
    def __init__(self):
        self.nc = bass.Bass("TRN2", target_bir_lowering=False)
        self.es = ExitStack()
        self.S = Sched(self.nc, self.es)
        self.rr = {}
        self.n = 0
        self.tmp = None

    def sb(self, shape, dt=F32, name=None):
        self.n += 1
        es = self.tmp if self.tmp is not None else self.es
        return es.enter_context(self.nc.sbuf_tensor(name or f"sb{self.n}", list(shape), dt))

    def push(self):
        self.tmp = ExitStack()

    def pop(self):
        self.barrier()
        self.tmp.close()
        self.tmp = None

    def barrier(self):
        S = self.S
        toks = []
        for st in S.st.values():
            if st['w']:
                toks.append(st['w'])
            toks.extend(st['r'])
        toks = [(a, b, 'x') for (a, b, c) in toks]
        for e in S.eng:
            S._waits(e, (), (), extra=toks)
        S.st = {}

    def ps(self, shape, dt=F32, name=None):
        self.n += 1
        es = self.tmp if self.tmp is not None else self.es
        return es.enter_context(self.nc.psum_tensor(name or f"ps{self.n}", list(shape), dt))

    def din(self, name, shape, dt=F32):
        return self.nc.dram_tensor(name, list(shape), dt, kind="ExternalInput")

    def dout(self, name, shape, dt=F32):
        return self.nc.dram_tensor(name, list(shape), dt, kind="ExternalOutput")

    def ring(self, key, items=None):
        if items is not None:
            self.rr[key] = [items, 0]
            return
        it, i = self.rr[key]
        self.rr[key][1] = (i + 1) % len(it)
        return it[i]

    def mm(self, out, lhsT, rhs, start, stop, reads, writes):
        self.S.op('pe', lambda e: e.matmul(out, lhsT=lhsT, rhs=rhs, start=start, stop=stop), reads, writes)

    def tr(self, out, in_, ident, reads, writes):
        self.S.op('pe', lambda e: e.transpose(out, in_, ident), reads, writes)

    def tt(self, eng, out, in0, in1, op, reads, writes):
        self.S.op(eng, lambda e: e.tensor_tensor(out=out, in0=in0, in1=in1, op=op), reads, writes)

    def ts(self, eng, out, in0, s1, s2, op0, op1, reads, writes, accum_out=None):
        if op1 is None:
            self.S.op(eng, lambda e: e.tensor_scalar(out=out, in0=in0, scalar1=s1, scalar2=None, op0=op0), reads, writes)
        elif accum_out is not None:
            self.S.op(eng, lambda e: e.tensor_scalar(out=out, in0=in0, scalar1=s1, scalar2=s2, op0=op0, op1=op1,
                                                      accum_out=accum_out), reads, writes)
        else:
            self.S.op(eng, lambda e: e.tensor_scalar(out=out, in0=in0, scalar1=s1, scalar2=s2, op0=op0, op1=op1), reads, writes)

    def stt(self, eng, out, in0, scalar, in1, op0, op1, reads, writes):
        self.S.op(eng, lambda e: e.scalar_tensor_tensor(out=out, in0=in0, scalar=scalar, in1=in1, op0=op0, op1=op1),
                  reads, writes)

    def act(self, out, in_, func, reads, writes, bias=None, scale=None, accum_out=None):
        kw = {}
        if bias is not None:
            kw['bias'] = bias
        if scale is not None:
            kw['scale'] = scale
        if accum_out is not None:
            kw['accum_out'] = accum_out
        self.S.op('act', lambda e: e.activation(out=out, in_=in_, func=func, **kw), reads, writes)

    def cp(self, eng, out, in_, reads, writes):
        if eng == 'act':
            self.S.op('act', lambda e: e.copy(out=out, in_=in_), reads, writes)
        else:
            self.S.op(eng, lambda e: e.tensor_copy(out=out, in_=in_), reads, writes)

    def memset(self, eng, ap, val, writes):
        self.S.op(eng, lambda e: e.memset(ap, val), (), writes)

    def dma(self, out, in_, reads, writes, q='sp'):
        self.S.dma(q, out, in_, reads, writes)

    def rsqrt(self, out, in_, reads_writes_tile, eps_mult=None):
        pass


def bc_mid(ap, n):
    sh = ap.shape
    return ap.unsqueeze(1).to_broadcast([sh[0], n, sh[1]])


def bc_last(ap, n):
    sh = ap.shape
    return ap.unsqueeze(2).to_broadcast([sh[0], sh[1], n])


def emit_mod(C, cT_d, adaw_d, adab_d, cols, outs):
    S = C.S
    cT = C.sb([128, 8])
    C.dma(cT[:], cT_d.ap()[:, :], [], [cT])
    cond = C.sb([128, 8])
    C.act(cond[:], cT[:], AF.Silu, [cT], [cond])
    crep = C.sb([128, 8, 128])
    C.cp('dve', crep[:], bc_last(cond[:], 128), [cond], [crep])
    wst = [C.sb([128, 8, 512]) for _ in range(2)]
    bst = [C.sb([128, 512]) for _ in range(2)]
    pm = C.ps([128, 512])
    for i, (blk, o) in enumerate(zip(cols, outs)):
        w = wst[i % 2]
        bb = bst[i % 2]
        C.dma(w[:], adaw_d.ap()[:, blk * 512:(blk + 1) * 512].rearrange("(kc p) n -> p kc n", p=128), [], [w])
        C.dma(bb[:], adab_d.ap()[:, blk * 512:(blk + 1) * 512], [], [bb])
        for kc in range(8):
            C.mm(pm[:], crep[:, kc, :], w[:, kc, :], kc == 0, kc == 7, [crep, w], [pm])
        C.tt('dve', o[0], pm[:], bb[:], ALU.add, [pm, bb], [o[1]])


def emit_norm_tile(C, xt, ntok, Grep, shrep, hb, tmp32, small):
    ss, = small
    C.memset('pool', ss[:ntok, 0:1], 0.0, [ss])
    C.act(tmp32[:ntok, :], xt[:ntok, :], AF.Square, [xt, ss], [tmp32, ss], accum_out=ss[:ntok, 0:1])
    C.ts('dve', ss[:ntok, 1:2], ss[:ntok, 0:1], 1.0 / 1024, 1e-6, ALU.mult, ALU.add, [ss], [ss])
    C.act(ss[:ntok, 2:3], ss[:ntok, 1:2], AF.Sqrt, [ss], [ss])
    C.S.op('dve', lambda e: e.reciprocal(out=ss[:ntok, 3:4], in_=ss[:ntok, 2:3]), [ss], [ss])
    C.stt('dve', tmp32[:ntok, :], xt[:ntok, :], ss[:ntok, 3:4], Grep[:ntok, :], ALU.mult, ALU.mult,
          [xt, ss, Grep], [tmp32])
    C.tt('pool', hb[:ntok, :], tmp32[:ntok, :], shrep[:ntok, :], ALU.add, [tmp32, shrep], [hb])


def emit_hT(C, hb, ntok, identb, pT, hT, col0):
    for kc in range(8):
        C.tr(pT[:, kc, :ntok], hb[:ntok, kc * 128:(kc + 1) * 128], identb[:ntok, :ntok], [hb, identb], [pT])
    C.cp('act', hT[:, :, col0:col0 + ntok], pT[:, :, :ntok], [pT], [hT])


def build_A(S, stage=99):
    C = Ctx()

    def done():
        C.S.finish('sp')
        C.es.close()
        return C.nc
    TW = 64
    NT = S // TW
    CPT = TW // 64
    NC1, NC2 = 2176, 1664
    x_d = C.din("x", [S, 1024])
    cT_d = C.din("cT", [128, 8])
    adaw_d = C.din("adaw", [1024, 3072])
    adab_d = C.din("adab", [128, 3072])
    g_d = C.din("g_rep", [128, 1024])
    W_d = C.din("W", [1024, NC1])
    mu_d = C.din("mu_rep", [128, NC2])
    pvec_d = C.din("pvec", [64, 5, 8])
    tvec_d = C.din("tvec", [64, 2, 512])
    wup_d = C.din("wup", [64, 2, 512])
    cst_d = C.din("cst", [128, 128 + TW])
    m64_d = C.din("m64", [64, 5, 64])
    yb_d = C.dout("yb", [S, 512])

    cst = C.sb([128, 128 + TW])
    C.dma(cst[:], cst_d.ap()[:, :], [], [cst])
    ident = cst[:, 0:128]
    identb = C.sb([128, 128], BF16)
    C.cp('dve', identb[:], ident, [cst], [identb])
    m64 = C.sb([64, 5, 64])
    C.dma(m64[:], m64_d.ap()[:, :, :], [], [m64])
    MUs, MUi, MLs, I64, ONES = (m64[:, i, :] for i in range(5))
    scanm = cst[0:64, 128:128 + TW]
    pvec = C.sb([64, 6, 8])
    C.dma(pvec[:, 0:5, :], pvec_d.ap()[:, :, :], [], [pvec])
    C.ts('dve', pvec[:, 5, :], pvec[:, 3, :], -1.0, 1.0, ALU.mult, ALU.add, [pvec], [pvec])
    tvec = C.sb([64, 2, 512])
    C.dma(tvec[:], tvec_d.ap()[:, :, :], [], [tvec])
    wup = C.sb([64, 2, 512])
    C.dma(wup[:], wup_d.ap()[:, :, :], [], [wup])

    Grep = C.sb([128, 1024])
    shrep = C.sb([128, 1024])
    W1 = C.sb([128, 8, NC1], BF16)
    W2 = C.sb([128, 8, NC2], BF16)
    C.push()
    emit_mod(C, cT_d, adaw_d, adab_d, [0, 1, 2, 3],
             [(shrep[:, 0:512], shrep), (shrep[:, 512:1024], shrep), (Grep[:, 0:512], Grep), (Grep[:, 512:1024], Grep)])
    gsb = C.sb([128, 1024])
    C.dma(gsb[:], g_d.ap()[:, :], [], [gsb])
    C.stt('dve', Grep[:], Grep[:], 1.0, gsb[:], ALU.add, ALU.mult, [Grep, gsb], [Grep])
    mu = C.sb([128, NC2])
    C.dma(mu[:], mu_d.ap()[:, :], [], [mu])
    stg = [C.sb([128, 8, 128]) for _ in range(2)]
    tmpw = [C.sb([128, 8, 128]) for _ in range(2)]
    for blk in range(NC1 // 128):
        st_ = stg[blk % 2]
        tw_ = tmpw[blk % 2]
        c0 = blk * 128
        C.dma(st_[:], W_d.ap()[:, c0:c0 + 128].rearrange("(kc p) n -> p kc n", p=128), [], [st_])
        if c0 < NC2:
            C.tt('dve', tw_[:], st_[:], bc_mid(mu[:, c0:c0 + 128], 8), ALU.mult, [st_, mu], [tw_])
            C.cp('act', W2[:, :, c0:c0 + 128], tw_[:], [tw_], [W2])
            C.tt('pool', W1[:, :, c0:c0 + 128], st_[:], tw_[:], ALU.subtract, [st_, tw_], [W1])
        else:
            C.cp('act', W1[:, :, c0:c0 + 128], st_[:], [st_], [W1])
    C.pop()
    if stage == 0:
        return done()

    xts = [C.sb([128, 1024]) for _ in range(2)]
    tmp32 = C.sb([128, 1024])
    hb = C.sb([128, 1024], BF16)
    ss = C.sb([128, 4])
    pT = C.ps([128, 8, 128], BF16)
    hTs = [C.sb([128, 8, 1 + TW], BF16) for _ in range(2)]
    PJ = [C.ps([128, 512]) for _ in range(2)]
    PC = [C.ps([128, 512]) for _ in range(5)]
    C.ring('pj', PJ)
    C.ring('pc', PC)

    def fm():
        return C.sb([64, 8, TW])
    r_sb, k_sb, t_lw, t_a, t_cs, t_kap, t_b, t_kt, t_e, t_d = (fm() for _ in range(10))
    wd_sb = C.sb([64, 2, TW])
    ops = dict(Rh=fm(), Kh=fm(), Bc=fm(), Kc=fm(), Bb=fm(), Kb=fm(), RKK=fm(), dec=C.sb([64, 8, CPT]))

    def pv(i):
        return bc_last(pvec[:, i, :], TW)

    ST = C.sb([64, 8, 64])
    C.memset('pool', ST[:], 0.0, [ST])
    STt = C.sb([64, 8, 64])

    def c64(n=1):
        return [C.sb([64, 8, 64]) for _ in range(n)]
    V_sb, SG_sb = c64(2), c64(2)
    Nn, Ll = c64(2), c64(2)
    AkT, MbT, MkT, Q, QT, RHS, negU, Ysb, BT, KT, Ytmp, Ysq = (c64()[0] for _ in range(12))
    gn = C.sb([64, 6, 8])
    dot = C.sb([64, 8])

    def f2(t):
        return t[:].rearrange("p h v -> p (h v)")

    for st in range(NT):
        hT = hTs[st % 2]
        hTp = hTs[(st + 1) % 2]
        xt = xts[st % 2]
        C.dma(xt[:TW, :], x_d.ap()[st * TW:(st + 1) * TW, :], [], [xt])
        emit_norm_tile(C, xt, TW, Grep, shrep, hb, tmp32, (ss,))
        emit_hT(C, hb, TW, identb, pT, hT, 1)
        if st == 0:
            C.memset('pool', hT[:, :, 0:1], 0.0, [hT])
        else:
            C.cp('pool', hT[:, :, 0:1], hTp[:, :, TW:TW + 1], [hTp], [hT])
        if stage == 1:
            return done()
        def proj(dst_ap, dst_res, j0, n, func=None):
            pj = C.ring('pj')
            for jj in range(n):
                c0 = (j0 + jj) * 64
                o = pj[0:64, jj * TW:(jj + 1) * TW]
                for kc in range(8):
                    C.mm(o, W1[:, kc, c0:c0 + 64], hT[:, kc, 1:1 + TW], kc == 0, False, [W1, hT], [pj])
                for kc in range(8):
                    C.mm(o, W2[:, kc, c0:c0 + 64], hT[:, kc, 0:TW], False, kc == 7, [W2, hT], [pj])
            if func is None:
                C.cp('act', dst_ap, pj[0:64, 0:n * TW].rearrange("p (h t) -> p h t", t=TW), [pj], [dst_res])
            return pj
        proj(r_sb[:, 0:4, :], r_sb, 0, 4)
        proj(r_sb[:, 4:8, :], r_sb, 4, 4)
        proj(k_sb[:, 0:4, :], k_sb, 8, 4)
        proj(k_sb[:, 4:8, :], k_sb, 12, 4)
        pj = proj(None, None, 16, 2, func='x')
        C.act(wd_sb[:, 0, :], pj[0:64, 0:TW], AF.Tanh, [pj], [wd_sb])
        C.cp('act', wd_sb[:, 1, :], pj[0:64, TW:2 * TW], [pj], [wd_sb])
        if stage == 2:
            return done()
        for half in range(2):
            pw = C.ring('pj')
            for hq in range(4):
                hl = half * 4 + hq
                C.mm(pw[0:64, hq * TW:(hq + 1) * TW], wup[:, 0, hl * 64:(hl + 1) * 64], wd_sb[:, 0, :], True, True, [wup, wd_sb], [pw])
            for hq in range(4):
                hl = half * 4 + hq
                C.act(t_lw[:, hl, :], pw[0:64, hq * TW:(hq + 1) * TW], AF.Sigmoid, [pw, pvec], [t_lw], bias=pvec[:, 0, hl:hl + 1])
            pa = C.ring('pj')
            for hq in range(4):
                hl = half * 4 + hq
                C.mm(pa[0:64, hq * TW:(hq + 1) * TW], wup[:, 1, hl * 64:(hl + 1) * 64], wd_sb[:, 1, :], True, True, [wup, wd_sb], [pa])
            for hq in range(4):
                hl = half * 4 + hq
                C.act(t_a[:, hl, :], pa[0:64, hq * TW:(hq + 1) * TW], AF.Sigmoid, [pa, pvec], [t_a], bias=pvec[:, 1, hl:hl + 1])
        C.ts('dve', t_lw[:], t_lw[:], -float(np.exp(-0.5)), None, ALU.mult, None, [t_lw], [t_lw])
        for hl in range(8):
            C.S.op('dve', lambda e: e.tensor_tensor_scan(out=t_cs[:, hl, :], data0=scanm, data1=t_lw[:, hl, :], initial=0.0,
                                                         op0=ALU.mult, op1=ALU.add), [cst, t_lw], [t_cs])
        C.tt('dve', t_kap[:], k_sb[:], pv(2), ALU.mult, [k_sb, pvec], [t_kap])
        C.act(t_e[:], t_kap[:], AF.Square, [t_kap], [t_e])
        for half in range(2):
            pn = C.ring('pj')
            C.mm(pn[0:64, 0:4 * TW], ONES, t_e[:, half * 4:(half + 1) * 4, :].rearrange("p h t -> p (h t)"), True, True, [m64, t_e], [pn])
            C.ts('dve', t_d[:, half * 4:(half + 1) * 4, :].rearrange("p h t -> p (h t)"), pn[0:64, 0:4 * TW], 1e-24, None, ALU.max, None,
                 [pn], [t_d])
        C.act(t_d[:], t_d[:], AF.Sqrt, [t_d], [t_d])
        C.S.op('dve', lambda e: e.reciprocal(out=t_d[:], in_=t_d[:]), [t_d], [t_d])
        C.tt('dve', t_kap[:], t_kap[:], t_d[:], ALU.mult, [t_kap, t_d], [t_kap])
        C.tt('pool', t_b[:], t_kap[:], t_a[:], ALU.mult, [t_kap, t_a], [t_b])
        C.tt('dve', t_kt[:], t_a[:], pv(3), ALU.mult, [t_a, pvec], [t_kt])
        C.tt('dve', t_kt[:], t_kt[:], pv(5), ALU.add, [t_kt, pvec], [t_kt])
        C.tt('dve', t_kt[:], t_kt[:], k_sb[:], ALU.mult, [t_kt, k_sb], [t_kt])
        C.tt('pool', ops['RKK'][:], r_sb[:], pv(4), ALU.mult, [r_sb, pvec], [ops['RKK']])
        C.tt('pool', ops['RKK'][:], ops['RKK'][:], t_kt[:], ALU.mult, [ops['RKK'], t_kt], [ops['RKK']])
        cs4 = t_cs[:].rearrange("p h (c t) -> p h c t", t=64)
        C.act(t_e[:], t_cs[:], AF.Exp, [t_cs], [t_e])
        C.tt('dve', ops['Rh'][:], r_sb[:], t_e[:], ALU.mult, [r_sb, t_e], [ops['Rh']])
        C.tt('pool', t_d[:], t_cs[:], t_lw[:], ALU.subtract, [t_cs, t_lw], [t_d])
        C.act(t_e[:], t_d[:], AF.Exp, [t_d], [t_e])
        C.tt('dve', ops['Kh'][:], t_kap[:], t_e[:], ALU.mult, [t_kap, t_e], [ops['Kh']])
        C.act(t_e[:], t_cs[:], AF.Exp, [t_cs], [t_e], scale=-1.0)
        C.tt('dve', ops['Bc'][:], t_b[:], t_e[:], ALU.mult, [t_b, t_e], [ops['Bc']])
        C.tt('pool', ops['Kc'][:], t_kt[:], t_e[:], ALU.mult, [t_kt, t_e], [ops['Kc']])
        for c in range(CPT):
            C.tt('dve', t_d[:, :, c * 64:(c + 1) * 64], bc_last(t_cs[:, :, c * 64 + 63], 64), t_cs[:, :, c * 64:(c + 1) * 64],
                 ALU.subtract, [t_cs], [t_d])
            C.act(ops['dec'][:, :, c], t_cs[:, :, c * 64 + 63], AF.Exp, [t_cs], [ops['dec']])
        C.act(t_e[:], t_d[:], AF.Exp, [t_d], [t_e])
        C.tt('dve', ops['Bb'][:], t_b[:], t_e[:], ALU.mult, [t_b, t_e], [ops['Bb']])
        C.tt('pool', ops['Kb'][:], t_kt[:], t_e[:], ALU.mult, [t_kt, t_e], [ops['Kb']])
        if stage == 3:
            return done()
        for ch in range(CPT):
            t0 = ch * 64
            gch = st * CPT + ch
            V = V_sb[gch % 2]
            SG = SG_sb[gch % 2]
            pvv = C.ring('pc')
            for kc in range(8):
                C.mm(pvv[0:64, :], hT[:, kc, 1 + t0:1 + t0 + 64], W1[:, kc, 1152:1664], kc == 0, False, [hT, W1], [pvv])
            for kc in range(8):
                C.mm(pvv[0:64, :], hT[:, kc, t0:t0 + 64], W2[:, kc, 1152:1664], False, kc == 7, [hT, W2], [pvv])
            C.cp('act', f2(V), pvv[0:64, :], [pvv], [V])
            pg = C.ring('pc')
            for kc in range(8):
                C.mm(pg[0:64, :], hT[:, kc, 1 + t0:1 + t0 + 64], W1[:, kc, 1664:2176], kc == 0, kc == 7, [hT, W1], [pg])
            C.act(f2(SG), pg[0:64, :], AF.Silu, [pg], [SG])
            if stage == 35:
                return done()

            def hs(name, hl):
                return ops[name][:, hl, t0:t0 + 64]
            N, L = Nn[0], Ll[0]
            specs = [("Bc", "Kh", MUs, N), ("Kh", "Bc", MLs, L), ("Kc", "Kh", MUs, AkT), ("Bc", "Rh", MUi, MbT),
                     ("Kc", "Rh", MUi, MkT)]
            for (la, ra, msk, dst) in specs:
                p = C.ring('pc')
                for hl in range(8):
                    C.mm(p[0:64, hl * 64:(hl + 1) * 64], hs(la, hl), hs(ra, hl), True, True, [ops[la], ops[ra]], [p])
                C.tt('dve', dst[:], p[0:64, :].rearrange("p (h v) -> p h v", v=64), bc_mid(msk, 8), ALU.mult,
                     [p, m64], [dst])
            if stage == 4:
                return done()
            C.tt('pool', Q[:], bc_mid(I64, 8), N[:], ALU.subtract, [m64, N], [Q])
            C.tt('pool', QT[:], bc_mid(I64, 8), L[:], ALU.subtract, [m64, L], [QT])
            for m in range(5):
                N2, L2 = Nn[(m + 1) % 2], Ll[(m + 1) % 2]
                p1 = C.ring('pc')
                p2 = C.ring('pc')
                for hl in range(8):
                    C.mm(p1[0:64, hl * 64:(hl + 1) * 64], L[:, hl, :], N[:, hl, :], True, True, [L, N], [p1])
                for hl in range(8):
                    C.mm(p2[0:64, hl * 64:(hl + 1) * 64], N[:, hl, :], L[:, hl, :], True, True, [L, N], [p2])
                C.cp('act', f2(N2), p1[0:64, :], [p1], [N2])
                C.cp('dve', f2(L2), p2[0:64, :], [p2], [L2])
                p3 = C.ring('pc')
                for hl in range(8):
                    C.mm(p3[0:64, hl * 64:(hl + 1) * 64], QT[:, hl, :], N2[:, hl, :], True, True, [QT, N2], [p3])
                if m < 4:
                    p4 = C.ring('pc')
                    for hl in range(8):
                        C.mm(p4[0:64, hl * 64:(hl + 1) * 64], N2[:, hl, :], QT[:, hl, :], True, True, [QT, N2], [p4])
                C.tt('dve', f2(Q), f2(Q), p3[0:64, :], ALU.add, [Q, p3], [Q])
                if m < 4:
                    C.tt('dve', f2(QT), f2(QT), p4[0:64, :], ALU.add, [QT, p4], [QT])
                N, L = N2, L2
            if stage == 5:
                return done()
            pb = C.ring('pc')
            for hl in range(8):
                C.tr(pb[0:64, hl * 64:(hl + 1) * 64], hs("Bb", hl), I64, [ops["Bb"], m64], [pb])
            C.cp('act', f2(BT), pb[0:64, :], [pb], [BT])
            pk = C.ring('pc')
            for hl in range(8):
                C.tr(pk[0:64, hl * 64:(hl + 1) * 64], hs("Kb", hl), I64, [ops["Kb"], m64], [pk])
            C.cp('act', f2(KT), pk[0:64, :], [pk], [KT])
            pd = C.ring('pc')
            for hl in range(8):
                C.mm(pd[0:64, hl:hl + 1], hs("RKK", hl), ONES[:, 0:1], True, True, [ops['RKK'], m64], [pd])
            C.cp('act', dot[:], pd[0:64, 0:8], [pd], [dot])
            if stage == 6:
                return done()
            pr = C.ring('pc')
            for hl in range(8):
                C.mm(pr[0:64, hl * 64:(hl + 1) * 64], hs("Kh", hl), ST[:, hl, :], True, False, [ops["Kh"], ST], [pr])
                C.mm(pr[0:64, hl * 64:(hl + 1) * 64], AkT[:, hl, :], V[:, hl, :], False, True, [AkT, V], [pr])
            C.cp('act', f2(RHS), pr[0:64, :], [pr], [RHS])
            pu = C.ring('pc')
            for hl in range(8):
                C.mm(pu[0:64, hl * 64:(hl + 1) * 64], Q[:, hl, :], RHS[:, hl, :], True, True, [Q, RHS], [pu])
            C.ts('dve', f2(negU), pu[0:64, :], -1.0, None, ALU.mult, None, [pu], [negU])
            py = C.ring('pc')
            for hl in range(8):
                C.mm(py[0:64, hl * 64:(hl + 1) * 64], hs("Rh", hl), ST[:, hl, :], True, False, [ops["Rh"], ST], [py])
                C.mm(py[0:64, hl * 64:(hl + 1) * 64], MbT[:, hl, :], negU[:, hl, :], False, False, [MbT, negU], [py])
                C.mm(py[0:64, hl * 64:(hl + 1) * 64], MkT[:, hl, :], V[:, hl, :], False, True, [MkT, V], [py])
            C.cp('act', f2(Ysb), py[0:64, :], [py], [Ysb])
            psS = C.ring('pc')
            for hl in range(8):
                C.mm(psS[0:64, hl * 64:(hl + 1) * 64], BT[:, hl, :], negU[:, hl, :], True, False, [BT, negU], [psS])
                C.mm(psS[0:64, hl * 64:(hl + 1) * 64], KT[:, hl, :], V[:, hl, :], False, True, [KT, V], [psS])
            C.tt('pool', STt[:], ST[:], bc_last(ops['dec'][:, :, ch], 64), ALU.mult, [ST, ops['dec']], [STt])
            C.tt('dve', f2(ST), f2(STt), psS[0:64, :], ALU.add, [STt, psS], [ST])
            if stage == 7:
                return done()
            C.S.op('dve', lambda e: e.tensor_reduce(out=gn[:, 0, :], in_=Ysb[:], axis=AX.X, op=ALU.add), [Ysb], [gn])
            C.act(Ysq[:], Ysb[:], AF.Square, [Ysb], [Ysq])
            C.S.op('dve', lambda e: e.tensor_reduce(out=gn[:, 1, :], in_=Ysq[:], axis=AX.X, op=ALU.add), [Ysq], [gn])
            C.ts('dve', gn[:, 2, :], gn[:, 0, :], 1.0 / 64, None, ALU.mult, None, [gn], [gn])
            C.tt('dve', gn[:, 3, :], gn[:, 2, :], gn[:, 2, :], ALU.mult, [gn], [gn])
            C.stt('dve', gn[:, 4, :], gn[:, 1, :], 1.0 / 64, gn[:, 3, :], ALU.mult, ALU.subtract, [gn], [gn])
            C.ts('dve', gn[:, 4, :], gn[:, 4, :], 64e-5, None, ALU.add, None, [gn], [gn])
            C.act(gn[:, 4, :], gn[:, 4, :], AF.Sqrt, [gn], [gn])
            C.S.op('dve', lambda e: e.reciprocal(out=gn[:, 5, :], in_=gn[:, 4, :]), [gn], [gn])
            C.tt('dve', Ytmp[:], Ysb[:], bc_last(gn[:, 2, :], 64), ALU.subtract, [Ysb, gn], [Ytmp])
            C.tt('pool', Ytmp[:], Ytmp[:], bc_last(gn[:, 5, :], 64), ALU.mult, [Ytmp, gn], [Ytmp])
            C.tt('dve', f2(Ytmp), f2(Ytmp), tvec[:, 0, :], ALU.mult, [Ytmp, tvec], [Ytmp])
            C.tt('pool', f2(Ytmp), f2(Ytmp), tvec[:, 1, :], ALU.add, [Ytmp, tvec], [Ytmp])
            C.tt('dve', Ysq[:], V[:], bc_last(dot[:], 64), ALU.mult, [V, dot], [Ysq])
            C.tt('pool', Ytmp[:], Ytmp[:], Ysq[:], ALU.add, [Ytmp, Ysq], [Ytmp])
            C.tt('dve', Ysq[:], Ytmp[:], SG[:], ALU.mult, [Ytmp, SG], [Ysq])
            tok0 = st * TW + t0
            C.dma(yb_d.ap()[tok0:tok0 + 64, :], f2(Ysq), [Ysq], [yb_d], q='pool')
    return done()


def host_consts_A(TW=64):
    cst = np.zeros((128, 128 + TW), np.float32)
    cst[:, 0:128] = np.eye(128, dtype=np.float32)
    sm = np.ones(TW, np.float32)
    sm[::64] = 0.0
    cst[:, 128:128 + TW] = sm[None, :]
    j = np.arange(64)[:, None]
    i = np.arange(64)[None, :]
    m64 = np.zeros((64, 5, 64), np.float32)
    m64[:, 0, :] = (j < i)
    m64[:, 1, :] = (j <= i)
    m64[:, 2, :] = (j > i)
    m64[:, 3, :] = np.eye(64)
    m64[:, 4, :] = 1.0
    return cst, m64


def rep(v, p=128):
    return np.ascontiguousarray(np.broadcast_to(np.asarray(v, np.float32)[None, :], (p, v.shape[0])))


def pcol(v, p=128):
    return np.ascontiguousarray(np.asarray(v, np.float32).reshape(-1, p).T)


def inputs_A(inp, b, hh, S):
    o = 512 * hh
    Win = inp["even_w_in"][0]
    W = np.concatenate([Win[:, 3072 + o:3072 + o + 512], Win[:, 4096 + o:4096 + o + 512], Win[:, 6144:6208],
                        Win[:, 6208:6272], Win[:, 5120 + o:5120 + o + 512], Win[:, 6272 + o:6272 + o + 512]], axis=1)
    mu_rkv = inp["rwkv_mu_rkv"][0]
    mu_l = inp["rwkv_mu_lora"][0]
    mu = np.concatenate([mu_rkv[0, o:o + 512], mu_rkv[1, o:o + 512], mu_l[0], mu_l[1], mu_rkv[2, o:o + 512]])
    vec = inp["rwkv_vec"][0]
    rk = inp["rwkv_r_k"][0].reshape(-1)
    pvec = np.stack([pcol(vec[0, o:o + 512], 64), pcol(vec[1, o:o + 512], 64), pcol(vec[2, o:o + 512], 64),
                     pcol(vec[3, o:o + 512], 64), pcol(rk[o:o + 512], 64)], axis=1)
    tvec = np.stack([rep(vec[4, o:o + 512], 64), rep(vec[5, o:o + 512], 64)], axis=1)
    wup = np.stack([inp["rwkv_w_up"][0][:, o:o + 512], inp["rwkv_a_up"][0][:, o:o + 512]], axis=1)
    cst, m64 = host_consts_A()
    return {"x": np.ascontiguousarray(inp["x"][b, :S]), "cT": pcol(inp["c"][b]), "adaw": np.ascontiguousarray(inp["ada_w"][0]),
            "adab": rep(inp["ada_b"][0]), "g_rep": rep(inp["norm_g"][0]), "W": np.ascontiguousarray(W),
            "mu_rep": rep(mu), "pvec": np.ascontiguousarray(pvec), "tvec": np.ascontiguousarray(tvec),
            "wup": np.ascontiguousarray(wup), "cst": cst, "m64": m64}


def run_A(inp, S, stage=99):
    nc = build_A(S, stage)
    maps = [inputs_A(inp, c // 2, c % 2, S) for c in range(NCORES)]
    res = run_bass_kernel_spmd(nc, maps, core_ids=list(range(NCORES)))
    yb = np.zeros((4, S, 1024), np.float32)
    for c in range(NCORES):
        yb[c // 2, :, 512 * (c % 2):512 * (c % 2) + 512] = res.results[c]["yb"]
    return yb


I32 = mybir.dt.int32
PI = float(np.pi)


def emit_sincos(C, SC, ang_src, pos_col, invall, kf, ki, n):
    C.ts('dve', SC[:, 0, :n], invall[:, :n], pos_col, None, ALU.mult, None, [invall, ang_src], [SC])
    C.ts('dve', SC[:, 1, :n], SC[:, 0, :n], 0.5 * PI, None, ALU.add, None, [SC], [SC])
    C.ts('dve', kf[:, :, :n], SC[:, :, :n], 1.0 / (2 * PI), None, ALU.mult, None, [SC], [kf])
    C.cp('dve', ki[:, :, :n], kf[:, :, :n], [kf], [ki])
    C.cp('dve', kf[:, :, :n], ki[:, :, :n], [ki], [kf])
    C.stt('dve', SC[:, :, :n], kf[:, :, :n], -2 * PI, SC[:, :, :n], ALU.mult, ALU.add, [kf, SC], [SC])
    C.ts('dve', SC[:, :, :n], SC[:, :, :n], -PI, PI, ALU.max, ALU.min, [SC], [SC])
    C.act(SC[:, :, :n], SC[:, :, :n], AF.Sin, [SC], [SC])


def emit_rope(C, dst, src, res_dst, res_src, SC, c0, half, tmp):
    cos = SC[:, 1, c0:c0 + half]
    sin = SC[:, 0, c0:c0 + half]
    x1, x2 = src[:, 0:half], src[:, half:2 * half]
    C.tt('dve', tmp[:, 0:half], x2, sin, ALU.mult, [res_src, SC], [tmp])
    C.tt('dve', dst[:, 0:half], x1, cos, ALU.mult, [res_src, SC], [res_dst])
    C.tt('dve', dst[:, 0:half], dst[:, 0:half], tmp[:, 0:half], ALU.subtract, [res_dst, tmp], [res_dst])
    C.tt('dve', tmp[:, 0:half], x1, sin, ALU.mult, [res_src, SC], [tmp])
    C.tt('dve', dst[:, half:2 * half], x2, cos, ALU.mult, [res_src, SC], [res_dst])
    C.tt('dve', dst[:, half:2 * half], dst[:, half:2 * half], tmp[:, 0:half], ALU.add, [res_dst, tmp], [res_dst])


def load_w_bf16(C, dst, src_d, nrow_chunks, ncols, blk=512):
    stg = [C.sb([128, nrow_chunks, blk]) for _ in range(2)]
    i = 0
    for c0 in range(0, ncols, blk):
        w = min(blk, ncols - c0)
        st_ = stg[i % 2]
        C.dma(st_[:, :, :w], src_d.ap()[:, c0:c0 + w].rearrange("(kc p) n -> p kc n", p=128), [], [st_])
        C.cp('act' if i % 2 == 0 else 'pool', dst[:, :, c0:c0 + w], st_[:, :, :w], [st_], [dst])
        i += 1


def build_B(S):
    C = Ctx()
    QS = S // 4
    TWc = 256
    NTS = QS // TWc
    x_d = C.din("x", [2, QS, 1024])
    xh_d = C.din("xh", [2, 32, 1024])
    hm_d = C.din("hm", [128, 2])
    yb_d = C.din("yb", [2, QS, 1024])
    cT_d = C.din("cT", [128, 8])
    adaw0_d = C.din("adaw0", [1024, 3072])
    adab0_d = C.din("adab0", [128, 3072])
    adaw1_d = C.din("adaw1", [1024, 3072])
    adab1_d = C.din("adab1", [128, 3072])
    g0_d = C.din("g0_rep", [128, 1024])
    g1_d = C.din("g1_rep", [128, 1024])
    Wc_d = C.din("Wc", [1024, 3072])
    Wo_d = C.din("Wo", [2048, 1024])
    Wkv_d = C.din("Wkv", [1024, 320])
    cw_d = C.din("cw", [128, 8, 31])
    cv_d = C.din("cv", [128, 3, 8])
    pos_d = C.din("pos", [128, 2, QS // 128], I32)
    inv_d = C.din("inv", [128, 96])
    cst_d = C.din("cst", [128, 256])
    x1_d = C.dout("x1", [2, QS, 1024])
    kT_d = C.dout("kT", [128, 2, QS])
    v_d = C.dout("v", [2, QS, 128])
    kiT_d = C.dout("kiT", [64, 2, QS])

    cst = C.sb([128, 256])
    C.dma(cst[:], cst_d.ap()[:, :], [], [cst])
    ident = cst[:, 0:128]
    onesM = cst[:, 128:256]
    identb = C.sb([128, 128], BF16)
    C.cp('dve', identb[:], ident, [cst], [identb])
    hm = C.sb([128, 2])
    C.dma(hm[:], hm_d.ap()[:, :], [], [hm])
    cw = C.sb([128, 8, 31])
    C.dma(cw[:], cw_d.ap()[:, :, :], [], [cw])
    cv = C.sb([128, 3, 8])
    C.dma(cv[:], cv_d.ap()[:, :, :], [], [cv])
    posi = C.sb([128, 2, QS // 128], I32)
    C.dma(posi[:], pos_d.ap()[:, :, :], [], [posi])
    posf = C.sb([128, 2, QS // 128])
    C.cp('dve', posf[:], posi[:], [posi], [posf])
    invall = C.sb([128, 96])
    C.dma(invall[:], inv_d.ap()[:, :], [], [invall])

    G0, sh0, gt0, G1, sh1 = (C.sb([128, 1024]) for _ in range(5))
    Wc = C.sb([128, 8, 3072], BF16)
    Wo = C.sb([128, 16, 1024], BF16)
    Wkv = C.sb([128, 8, 320], BF16)
    C.push()
    emit_mod(C, cT_d, adaw0_d, adab0_d, [0, 1, 2, 3, 4, 5],
             [(sh0[:, 0:512], sh0), (sh0[:, 512:1024], sh0), (G0[:, 0:512], G0), (G0[:, 512:1024], G0),
              (gt0[:, 0:512], gt0), (gt0[:, 512:1024], gt0)])
    C.pop()
    C.push()
    emit_mod(C, cT_d, adaw1_d, adab1_d, [0, 1, 2, 3],
             [(sh1[:, 0:512], sh1), (sh1[:, 512:1024], sh1), (G1[:, 0:512], G1), (G1[:, 512:1024], G1)])
    gsb = C.sb([128, 1024])
    C.dma(gsb[:], g0_d.ap()[:, :], [], [gsb])
    C.stt('dve', G0[:], G0[:], 1.0, gsb[:], ALU.add, ALU.mult, [G0, gsb], [G0])
    gsb1 = C.sb([128, 1024])
    C.dma(gsb1[:], g1_d.ap()[:, :], [], [gsb1])
    C.stt('dve', G1[:], G1[:], 1.0, gsb1[:], ALU.add, ALU.mult, [G1, gsb1], [G1])
    C.pop()
    C.push()
    load_w_bf16(C, Wc, Wc_d, 8, 3072)
    load_w_bf16(C, Wo, Wo_d, 16, 1024, blk=256)
    load_w_bf16(C, Wkv, Wkv_d, 8, 320)
    C.pop()

    xts = [C.sb([128, 1024]) for _ in range(2)]
    ybt = C.sb([128, 1024])
    ybb = C.sb([128, 1024], BF16)
    x1t = C.sb([128, 1024])
    tmp32 = C.sb([128, 1024])
    hb = C.sb([128, 1024], BF16)
    ss = C.sb([128, 4])
    hT = C.sb([128, 8, TWc], BF16)
    hTh = C.sb([128, 8, 32], BF16)
    h1T = C.sb([128, 8, 128], BF16)
    ybT = C.sb([128, 8, 128], BF16)
    U = C.sb([128, 8, 30 + TWc])
    uh = C.sb([128, 8, 32])
    SG = C.sb([128, 8, TWc])
    acc = C.sb([128, 8, TWc])
    sqb = C.sb([128, 8, TWc])
    sig = C.sb([128, TWc])
    mean, msq, rstd = (C.sb([128, TWc]) for _ in range(3))
    yaT = C.sb([128, 8, TWc], BF16)
    kvs = C.sb([128, 320])
    krot = C.sb([128, 192])
    rtmp = C.sb([128, 64])
    SC = C.sb([128, 2, 96])
    kf = C.sb([128, 2, 96])
    kint = C.sb([128, 2, 96], I32)
    kTs = C.sb([128, 128])
    kiTs = C.sb([64, 128])
    pT = C.ps([128, 8, 128], BF16)
    C.ring('fm', [C.ps([128, 512]) for _ in range(3)])
    C.ring('po', [C.ps([128, 512]) for _ in range(2)])
    pm = C.ps([128, 512])
    pq = C.ps([128, 512])

    def glu_block(hTsrc, n, u_dst_fn):
        for cc in range(8):
            pv_ = C.ring('fm')
            pg_ = C.ring('fm')
            for kc in range(8):
                C.mm(pv_[:, :n], Wc[:, kc, cc * 128:(cc + 1) * 128], hTsrc[:, kc, :n], kc == 0, kc == 7, [Wc, hTsrc], [pv_])
            for kc in range(8):
                C.mm(pg_[:, :n], Wc[:, kc, 1024 + cc * 128:1024 + (cc + 1) * 128], hTsrc[:, kc, :n], kc == 0, kc == 7, [Wc, hTsrc], [pg_])
            C.act(sig[:, :n], pg_[:, :n], AF.Sigmoid, [pg_], [sig])
            dst, res = u_dst_fn(cc)
            C.tt('dve', dst, pv_[:, :n], sig[:, :n], ALU.mult, [pv_, sig], [res])

    for sg in range(2):
        C.dma(xts[0][:32, :], xh_d.ap()[sg, :, :], [], [xts[0]])
        emit_norm_tile(C, xts[0], 32, G0, sh0, hb, tmp32, (ss,))
        emit_hT(C, hb, 32, identb, pT, hTh, 0)
        glu_block(hTh, 32, lambda cc: (uh[:, cc, :], uh))
        C.ts('dve', U[:, :, 0:30], uh[:, :, 2:32], hm[:, sg:sg + 1], None, ALU.mult, None, [uh, hm], [U])
        for ti in range(NTS):
            if ti > 0:
                C.cp('pool', U[:, :, 0:30], U[:, :, TWc:TWc + 30], [U], [U])
            for sub in range(2):
                tok0 = ti * TWc + sub * 128
                C.dma(xts[sub][:], x_d.ap()[sg, tok0:tok0 + 128, :], [], [xts[sub]])
                emit_norm_tile(C, xts[sub], 128, G0, sh0, hb, tmp32, (ss,))
                emit_hT(C, hb, 128, identb, pT, hT, sub * 128)
            glu_block(hT, TWc, lambda cc: (U[:, cc, 30:30 + TWc], U))
            for cc in range(8):
                pg_ = C.ring('fm')
                for kc in range(8):
                    C.mm(pg_[:, :TWc], Wc[:, kc, 2048 + cc * 128:2048 + (cc + 1) * 128], hT[:, kc, :], kc == 0, kc == 7, [Wc, hT], [pg_])
                C.act(SG[:, cc, :], pg_[:, :TWc], AF.Silu, [pg_], [SG])
            for cc in range(8):
                C.ts('dve', acc[:, cc, :], U[:, cc, 0:TWc], cw[:, cc, 0:1], cv[:, 0, cc:cc + 1], ALU.mult, ALU.add, [U, cw, cv], [acc])
                for j in range(1, 31):
                    C.stt('dve', acc[:, cc, :], U[:, cc, j:j + TWc], cw[:, cc, j:j + 1], acc[:, cc, :], ALU.mult, ALU.add,
                          [U, cw, acc], [acc])
            C.act(sqb[:], acc[:], AF.Square, [acc], [sqb])
            for cc in range(8):
                C.mm(pm[:, :TWc], onesM, acc[:, cc, :], cc == 0, cc == 7, [cst, acc], [pm])
            for cc in range(8):
                C.mm(pq[:, :TWc], onesM, sqb[:, cc, :], cc == 0, cc == 7, [cst, sqb], [pq])
            C.cp('act', mean[:], pm[:, :TWc], [pm], [mean])
            C.tt('dve', msq[:], mean[:], mean[:], ALU.mult, [mean], [msq])
            C.tt('dve', rstd[:], pq[:, :TWc], msq[:], ALU.subtract, [pq, msq], [rstd])
            C.ts('dve', rstd[:], rstd[:], 1e-5, None, ALU.add, None, [rstd], [rstd])
            C.act(rstd[:], rstd[:], AF.Sqrt, [rstd], [rstd])
            C.S.op('dve', lambda e: e.reciprocal(out=rstd[:], in_=rstd[:]), [rstd], [rstd])
            C.tt('dve', sqb[:], acc[:], bc_mid(mean[:], 8), ALU.subtract, [acc, mean], [sqb])
            C.tt('pool', sqb[:], sqb[:], bc_mid(rstd[:], 8), ALU.mult, [sqb, rstd], [sqb])
            for cc in range(8):
                C.act(sqb[:, cc, :], sqb[:, cc, :], AF.Silu, [sqb, cv], [sqb], bias=cv[:, 2, cc:cc + 1], scale=cv[:, 1, cc:cc + 1])
            C.tt('dve', yaT[:], sqb[:], SG[:], ALU.mult, [sqb, SG], [yaT])
            for sub in range(2):
                tok0 = ti * TWc + sub * 128
                xt = xts[sub]
                C.dma(ybt[:], yb_d.ap()[sg, tok0:tok0 + 128, :], [], [ybt])
                C.cp('act', ybb[:], ybt[:], [ybt], [ybb])
                emit_hT(C, ybb, 128, identb, pT, ybT, 0)
                for half in range(2):
                    po = C.ring('po')
                    for cc in range(8):
                        C.mm(po[:], yaT[:, cc, sub * 128:(sub + 1) * 128], Wo[:, cc, half * 512:(half + 1) * 512], cc == 0, False, [yaT, Wo], [po])
                    for cc in range(8):
                        C.mm(po[:], ybT[:, cc, :], Wo[:, 8 + cc, half * 512:(half + 1) * 512], False, cc == 7, [ybT, Wo], [po])
                    C.tt('dve', tmp32[:, half * 512:(half + 1) * 512], po[:], gt0[:, half * 512:(half + 1) * 512], ALU.mult, [po, gt0], [tmp32])
                C.tt('pool', x1t[:], tmp32[:], xt[:], ALU.add, [tmp32, xt], [x1t])
                C.dma(x1_d.ap()[sg, tok0:tok0 + 128, :], x1t[:], [x1t], [x1_d], q='pool')
                emit_norm_tile(C, x1t, 128, G1, sh1, hb, tmp32, (ss,))
                emit_hT(C, hb, 128, identb, pT, h1T, 0)
                pk = C.ring('po')
                for kc in range(8):
                    C.mm(pk[:, 0:320], h1T[:, kc, :], Wkv[:, kc, :], kc == 0, kc == 7, [h1T, Wkv], [pk])
                C.cp('act', kvs[:], pk[:, 0:320], [pk], [kvs])
                gt = (ti * TWc + sub * 128) // 128
                emit_sincos(C, SC, posf, posf[:, sg, gt:gt + 1], invall, kf, kint, 96)
                emit_rope(C, krot[:, 0:128], kvs[:, 0:128], krot, kvs, SC, 0, 64, rtmp)
                emit_rope(C, krot[:, 128:192], kvs[:, 256:320], krot, kvs, SC, 64, 32, rtmp)
                pt1 = C.ring('po')
                C.tr(pt1[:, 0:128], krot[:, 0:128], ident, [krot, cst], [pt1])
                C.cp('act', kTs[:], pt1[:, 0:128], [pt1], [kTs])
                C.dma(kT_d.ap()[:, sg, tok0:tok0 + 128], kTs[:], [kTs], [kT_d], q='pool')
                pt2 = C.ring('po')
                C.tr(pt2[0:64, 0:128], krot[:, 128:192], ident, [krot, cst], [pt2])
                C.cp('act', kiTs[:], pt2[0:64, 0:128], [pt2], [kiTs])
                C.dma(kiT_d.ap()[:, sg, tok0:tok0 + 128], kiTs[:], [kiTs], [kiT_d], q='pool')
                C.dma(v_d.ap()[sg, tok0:tok0 + 128, :], kvs[:, 128:256], [kvs], [v_d], q='pool')
    C.S.finish('sp')
    C.es.close()
    return C.nc


QUARTERS = {0: (0, 3), 1: (1, 2)}


def rope_inv():
    i128 = (10000.0 ** (-np.arange(0, 128, 2, dtype=np.float32) / np.float32(128))).astype(np.float32)
    i64 = (10000.0 ** (-np.arange(0, 64, 2, dtype=np.float32) / np.float32(64))).astype(np.float32)
    return rep(np.concatenate([i128, i64]))


def inputs_B(inp, yb, b, hh, S):
    QS = S // 4
    qs = QUARTERS[hh]
    x = inp["x"][b, :S]
    xs = np.stack([x[q * QS:(q + 1) * QS] for q in qs])
    xh = np.stack([x[q * QS - 32:q * QS] if q > 0 else np.zeros((32, 1024), np.float32) for q in qs])
    hm = np.zeros((128, 2), np.float32)
    for i, q in enumerate(qs):
        hm[:, i] = 1.0 if q > 0 else 0.0
    ybs = np.stack([yb[b, q * QS:(q + 1) * QS] for q in qs])
    Win = inp["even_w_in"][0]
    Wod = inp["odd_w_in"][0]
    Wkv = np.concatenate([Wod[:, 2048:2304], Wod[:, 2816:2880]], axis=1)
    cw = inp["conv_w"][0][:, 0, :]
    cwp = np.ascontiguousarray(cw.T.reshape(8, 128, 31).transpose(1, 0, 2))
    cvec = inp["conv_vec"][0]
    cvp = np.ascontiguousarray(cvec.reshape(3, 8, 128).transpose(2, 0, 1))
    pos = inp["positions"][b, :S]
    posq = np.stack([pos[q * QS:(q + 1) * QS] for q in qs])
    posp = np.ascontiguousarray(posq.reshape(2, QS // 128, 128).transpose(2, 0, 1)).astype(np.int32)
    cst = np.zeros((128, 256), np.float32)
    cst[:, 0:128] = np.eye(128)
    cst[:, 128:256] = 1.0 / 1024
    return {"x": np.ascontiguousarray(xs), "xh": np.ascontiguousarray(xh), "hm": hm, "yb": np.ascontiguousarray(ybs),
            "cT": pcol(inp["c"][b]), "adaw0": np.ascontiguousarray(inp["ada_w"][0]), "adab0": rep(inp["ada_b"][0]),
            "adaw1": np.ascontiguousarray(inp["ada_w"][1]), "adab1": rep(inp["ada_b"][1]),
            "g0_rep": rep(inp["norm_g"][0]), "g1_rep": rep(inp["norm_g"][1]),
            "Wc": np.ascontiguousarray(Win[:, 0:3072]), "Wo": np.ascontiguousarray(inp["even_w_out"][0]),
            "Wkv": np.ascontiguousarray(Wkv), "cw": cwp, "cv": cvp, "pos": posp, "inv": rope_inv(), "cst": cst}


def run_B(inp, yb, S):
    nc = build_B(S)
    maps = [inputs_B(inp, yb, c // 2, c % 2, S) for c in range(NCORES)]
    res = run_bass_kernel_spmd(nc, maps, core_ids=list(range(NCORES)))
    QS = S // 4
    x1 = np.zeros((4, S, 1024), np.float32)
    kT = np.zeros((4, 128, S), np.float32)
    v = np.zeros((4, S, 128), np.float32)
    kiT = np.zeros((4, 64, S), np.float32)
    for c in range(NCORES):
        b, hh = c // 2, c % 2
        r = res.results[c]
        for i, q in enumerate(QUARTERS[hh]):
            x1[b, q * QS:(q + 1) * QS] = r["x1"][i]
            kT[b, :, q * QS:(q + 1) * QS] = r["kT"][:, i, :]
            v[b, q * QS:(q + 1) * QS] = r["v"][i]
            kiT[b, :, q * QS:(q + 1) * QS] = r["kiT"][:, i, :]
    return x1, kT, v, kiT


def build_C1(S):
    C = Ctx()
    QS = S // 4
    NBS = QS // 128
    NB = 2 * NBS
    x1_d = C.din("x1", [2, QS, 1024])
    cT_d = C.din("cT", [128, 8])
    adaw1_d = C.din("adaw1", [1024, 3072])
    adab1_d = C.din("adab1", [128, 3072])
    g1_d = C.din("g1_rep", [128, 1024])
    Wq_d = C.din("Wq", [1024, 4616])
    pos_d = C.din("pos", [128, 2, NBS], I32)
    inv_d = C.din("inv", [128, 96])
    cst_d = C.din("cst", [128, 256])
    QT_d = C.dout("QT", [128, NB, 16, 128], BF16)
    QiT_d = C.dout("QiT", [64, NB, 8, 128], BF16)
    sg_d = C.dout("sg", [NB, 128, 2048], BF16)
    wi_d = C.dout("wi", [128, NB, 8])

    cst = C.sb([128, 256])
    C.dma(cst[:], cst_d.ap()[:, :], [], [cst])
    identb = C.sb([128, 128], BF16)
    C.cp('dve', identb[:], cst[:, 0:128], [cst], [identb])
    posi = C.sb([128, 2, NBS], I32)
    C.dma(posi[:], pos_d.ap()[:, :, :], [], [posi])
    posf = C.sb([128, 2, NBS])
    C.cp('dve', posf[:], posi[:], [posi], [posf])
    invall = C.sb([128, 96])
    C.dma(invall[:], inv_d.ap()[:, :], [], [invall])
    G1, sh1 = C.sb([128, 1024]), C.sb([128, 1024])
    Wq = C.sb([128, 8, 4616], BF16)
    C.push()
    emit_mod(C, cT_d, adaw1_d, adab1_d, [0, 1, 2, 3],
             [(sh1[:, 0:512], sh1), (sh1[:, 512:1024], sh1), (G1[:, 0:512], G1), (G1[:, 512:1024], G1)])
    gsb1 = C.sb([128, 1024])
    C.dma(gsb1[:], g1_d.ap()[:, :], [], [gsb1])
    C.stt('dve', G1[:], G1[:], 1.0, gsb1[:], ALU.add, ALU.mult, [G1, gsb1], [G1])
    C.pop()
    C.push()
    load_w_bf16(C, Wq, Wq_d, 8, 4616)
    C.pop()

    xt = C.sb([128, 1024])
    tmp32 = C.sb([128, 1024])
    hb = C.sb([128, 1024], BF16)
    ss = C.sb([128, 4])
    h1T = C.sb([128, 8, 128], BF16)
    SC = C.sb([128, 2, 96])
    kf = C.sb([128, 2, 96])
    kint = C.sb([128, 2, 96], I32)
    qf = C.sb([128, 4, 128])
    t1 = C.sb([128, 4, 64])
    qb = C.sb([128, 4, 128], BF16)
    qif = C.sb([128, 8, 64])
    t2 = C.sb([128, 8, 32])
    qib = C.sb([128, 8, 64], BF16)
    QTs = C.sb([128, 16, 128], BF16)
    QiTs = C.sb([64, 8, 128], BF16)
    sgt = C.sb([128, 2048], BF16)
    wis = C.sb([128, 8])
    pT = C.ps([128, 8, 128], BF16)
    C.ring('pp', [C.ps([128, 512]) for _ in range(3)])

    def rope_multi(dst, src, nh, half, c0, tmp, res_dst, res_src):
        cos = bc_mid(SC[:, 1, c0:c0 + half], nh)
        sin = bc_mid(SC[:, 0, c0:c0 + half], nh)
        x1, x2 = src[:, :, 0:half], src[:, :, half:2 * half]
        C.tt('dve', tmp[:], x2, sin, ALU.mult, [res_src, SC], [tmp])
        C.tt('pool', dst[:, :, 0:half], x1, cos, ALU.mult, [res_src, SC], [res_dst])
        C.tt('dve', dst[:, :, 0:half], dst[:, :, 0:half], tmp[:], ALU.subtract, [res_dst, tmp], [res_dst])
        C.tt('dve', tmp[:], x1, sin, ALU.mult, [res_src, SC], [tmp])
        C.tt('pool', dst[:, :, half:2 * half], x2, cos, ALU.mult, [res_src, SC], [res_dst])
        C.tt('dve', dst[:, :, half:2 * half], dst[:, :, half:2 * half], tmp[:], ALU.add, [res_dst, tmp], [res_dst])

    for sg in range(2):
        for blk in range(NBS):
            nb = sg * NBS + blk
            C.dma(xt[:], x1_d.ap()[sg, blk * 128:(blk + 1) * 128, :], [], [xt])
            emit_norm_tile(C, xt, 128, G1, sh1, hb, tmp32, (ss,))
            emit_hT(C, hb, 128, identb, pT, h1T, 0)
            emit_sincos(C, SC, posf, posf[:, sg, blk:blk + 1], invall, kf, kint, 96)
            for grp in range(4):
                pq_ = C.ring('pp')
                for kc in range(8):
                    C.mm(pq_[:], h1T[:, kc, :], Wq[:, kc, grp * 512:(grp + 1) * 512], kc == 0, kc == 7, [h1T, Wq], [pq_])
                C.cp('act', qf[:].rearrange("p h d -> p (h d)"), pq_[:], [pq_], [qf])
                rope_multi(qb, qf, 4, 64, 0, t1, qb, qf)
                for h in range(4):
                    C.tr(pT[:, h, :], qb[:, h, :], identb[:], [qb, identb], [pT])
                C.cp('act', QTs[:, grp * 4:(grp + 1) * 4, :], pT[:, 0:4, :], [pT], [QTs])
            C.dma(QT_d.ap()[:, nb, :, :], QTs[:], [QTs], [QT_d], q='pool')
            pi_ = C.ring('pp')
            for kc in range(8):
                C.mm(pi_[:], h1T[:, kc, :], Wq[:, kc, 2048:2560], kc == 0, kc == 7, [h1T, Wq], [pi_])
            C.cp('act', qif[:].rearrange("p h d -> p (h d)"), pi_[:], [pi_], [qif])
            rope_multi(qib, qif, 8, 32, 64, t2, qib, qif)
            for h in range(8):
                C.tr(pT[0:64, h, :], qib[:, h, :], identb[:], [qib, identb], [pT])
            C.cp('act', QiTs[:], pT[0:64, :, :], [pT], [QiTs])
            C.dma(QiT_d.ap()[:, nb, :, :], QiTs[:], [QiTs], [QiT_d], q='pool')
            pw_ = C.ring('pp')
            for kc in range(8):
                C.mm(pw_[:, 0:8], h1T[:, kc, :], Wq[:, kc, 2560:2568], kc == 0, kc == 7, [h1T, Wq], [pw_])
            C.ts('dve', wis[:], pw_[:, 0:8], float(64 ** -0.5 * 8 ** -0.5), None, ALU.mult, None, [pw_], [wis])
            C.dma(wi_d.ap()[:, nb, :], wis[:], [wis], [wi_d], q='pool')
            for grp in range(4):
                pg_ = C.ring('pp')
                for kc in range(8):
                    C.mm(pg_[:], h1T[:, kc, :], Wq[:, kc, 2568 + grp * 512:2568 + (grp + 1) * 512], kc == 0, kc == 7, [h1T, Wq], [pg_])
                C.act(sgt[:, grp * 512:(grp + 1) * 512], pg_[:], AF.Silu, [pg_], [sgt])
            C.dma(sg_d.ap()[nb, :, :], sgt[:], [sgt], [sg_d], q='pool')
    C.S.finish('sp')
    C.es.close()
    return C.nc


def inputs_C1(inp, x1, b, hh, S):
    QS = S // 4
    qs = QUARTERS[hh]
    x1s = np.stack([x1[b, q * QS:(q + 1) * QS] for q in qs])
    Wod = inp["odd_w_in"][0]
    Wq = np.concatenate([Wod[:, 0:2048], Wod[:, 2304:2816], Wod[:, 2880:2888], Wod[:, 2888:4936]], axis=1)
    pos = inp["positions"][b, :S]
    posq = np.stack([pos[q * QS:(q + 1) * QS] for q in qs])
    posp = np.ascontiguousarray(posq.reshape(2, QS // 128, 128).transpose(2, 0, 1)).astype(np.int32)
    cst = np.zeros((128, 256), np.float32)
    cst[:, 0:128] = np.eye(128)
    cst[:, 128:256] = 1.0 / 1024
    return {"x1": np.ascontiguousarray(x1s), "cT": pcol(inp["c"][b]), "adaw1": np.ascontiguousarray(inp["ada_w"][1]),
            "adab1": rep(inp["ada_b"][1]), "g1_rep": rep(inp["norm_g"][1]), "Wq": np.ascontiguousarray(Wq),
            "pos": posp, "inv": rope_inv(), "cst": cst}


def run_C1(inp, x1, S):
    nc = build_C1(S)
    maps = [inputs_C1(inp, x1, c // 2, c % 2, S) for c in range(NCORES)]
    res = run_bass_kernel_spmd(nc, maps, core_ids=list(range(NCORES)))
    return [res.results[c] for c in range(NCORES)]


MASKV = -30000.0
NIT = 30


def build_C2(S, dbg=False):
    C = Ctx()
    QS = S // 4
    NBS = QS // 128
    NB = 2 * NBS
    KOFF = (QS, 3 * QS)
    QT_d = C.din("QT", [128, NB, 16, 128], BF16)
    QiT_d = C.din("QiT", [64, NB, 8, 128], BF16)
    sg_d = C.din("sg", [NB, 128, 2048], BF16)
    wi_d = C.din("wi", [128, NB, 8])
    x1_d = C.din("x1", [2, QS, 1024])
    kT_d = C.din("kT", [128, S])
    kiT_d = C.din("kiT", [64, S])
    v_d = C.din("v", [S, 128])
    qc_d = C.din("qc", [128, NB])
    kc_d = C.din("kc512", [128, 512])
    Wo_d = C.din("Wo1", [2048, 1024])
    cT_d = C.din("cT", [128, 8])
    adaw1_d = C.din("adaw1", [1024, 3072])
    adab1_d = C.din("adab1", [128, 3072])
    fg_d = C.din("fg_rep", [128, 1024])
    cst_d = C.din("cst", [128, 256])
    out_d = C.dout("out", [2, QS, 1024])
    if dbg:
        odbg_d = C.dout("odbg", [NB, 128, 2048])
        cdbg_d = C.dout("cdbg", [128, NB, 4])
        cdb = C.sb([128, NB, 4])
        C.memset('pool', cdb[:], 0.0, [cdb])

    cst = C.sb([128, 256])
    C.dma(cst[:], cst_d.ap()[:, :], [], [cst])
    identb = C.sb([128, 128], BF16)
    C.cp('dve', identb[:], cst[:, 0:128], [cst], [identb])
    onesb = C.sb([128, 128], BF16)
    C.memset('pool', onesb[:], 1.0, [onesb])
    kc512 = C.sb([128, 512])
    C.dma(kc512[:], kc_d.ap()[:, :], [], [kc512])
    qc = C.sb([128, NB])
    C.dma(qc[:], qc_d.ap()[:, :], [], [qc])
    wiall = C.sb([128, NB, 8])
    C.dma(wiall[:], wi_d.ap()[:, :, :], [], [wiall])
    fg = C.sb([128, 1024])
    C.dma(fg[:], fg_d.ap()[:, :], [], [fg])
    gt1 = C.sb([128, 1024])
    kTb = C.sb([128, S], BF16)
    kiTb = C.sb([64, S], BF16)
    Vb = C.sb([128, S // 128, 128], BF16)
    Wo = C.sb([128, 16, 1024], BF16)
    C.push()
    emit_mod(C, cT_d, adaw1_d, adab1_d, [4, 5], [(gt1[:, 0:512], gt1), (gt1[:, 512:1024], gt1)])
    C.pop()
    C.push()
    load_w_bf16(C, Wo, Wo_d, 16, 1024, blk=256)
    stg = [C.sb([128, 1024]) for _ in range(2)]
    i = 0
    for c0 in range(0, S, 1024):
        w = min(1024, S - c0)
        s_ = stg[i % 2]
        C.dma(s_[:, :w], kT_d.ap()[:, c0:c0 + w], [], [s_])
        C.cp('act', kTb[:, c0:c0 + w], s_[:, :w], [s_], [kTb])
        i += 1
        s_ = stg[i % 2]
        C.dma(s_[0:64, :w], kiT_d.ap()[:, c0:c0 + w], [], [s_])
        C.cp('pool', kiTb[:, c0:c0 + w], s_[0:64, :w], [s_], [kiTb])
        i += 1
        s_ = stg[i % 2]
        nkb_ = w // 128
        C.dma(s_[:, :w].rearrange("p (kb d) -> p kb d", d=128),
              v_d.ap()[c0:c0 + w, :].rearrange("(kb p) d -> p kb d", p=128), [], [s_])
        C.cp('act', Vb[:, c0 // 128:c0 // 128 + nkb_, :], s_[:, :w].rearrange("p (kb d) -> p kb d", d=128), [s_], [Vb])
        i += 1
    C.pop()

    score = C.sb([128, S])
    Mb = C.sb([128, S], BF16)
    QTs = C.sb([128, 16, 128], BF16)
    QiTs = C.sb([64, 8, 128], BF16)
    sgt = C.sb([128, 2048], BF16)
    xt = C.sb([128, 1024])
    x2 = C.sb([128, 1024])
    tmp32 = C.sb([128, 1024])
    ot = C.sb([128, 1024])
    ss = C.sb([128, 4])
    C.ring('rl', [C.sb([128, 512]) for _ in range(2)])
    mbias = C.sb([128, 512])
    bs = C.sb([128, 8])
    cnt = C.sb([128, NIT])
    qcj = C.sb([128, 1])
    C.ring('MT', [C.sb([128, 8, 128], BF16) for _ in range(2)])
    C.ring('E', [C.sb([128, 4, 128], BF16) for _ in range(3)])
    C.ring('P', [C.sb([128, 4, 128], BF16) for _ in range(3)])
    tmpo = C.sb([128, 512])
    sgT = C.sb([128, 16, 128], BF16)
    ogT = C.sb([128, 16, 128], BF16)
    pTb = C.ps([128, 8, 128], BF16)
    ACC = [C.ps([128, 512]) for _ in range(2)]
    RS = [C.ps([128, 512]) for _ in range(2)]
    C.ring('st', [C.ps([128, 512]) for _ in range(2)])
    SCALE = float(128 ** -0.5)

    for sgi in range(2):
        for blk in range(NBS):
            nb = sgi * NBS + blk
            nk = KOFF[sgi] + (blk + 1) * 128
            nkb = nk // 128
            C.dma(QTs[:], QT_d.ap()[:, nb, :, :], [], [QTs])
            C.dma(QiTs[:], QiT_d.ap()[:, nb, :, :], [], [QiTs])
            C.dma(sgt[:], sg_d.ap()[nb, :, :], [], [sgt])
            C.dma(xt[:], x1_d.ap()[sgi, blk * 128:(blk + 1) * 128, :], [], [xt])
            for j in range((nk + 511) // 512):
                w = min(512, nk - j * 512)
                cols = slice(j * 512, j * 512 + w)
                for h in range(8):
                    ps_ = C.ring('st')
                    C.mm(ps_[:, :w], QiTs[:, h, :], kiTb[:, cols], True, True, [QiTs, kiTb], [ps_])
                    rl = C.ring('rl')
                    C.act(rl[:, :w], ps_[:, :w], AF.Relu, [ps_], [rl])
                    if h == 0:
                        C.ts('dve', score[:, cols], rl[:, :w], wiall[:, nb, 0:1], None, ALU.mult, None, [rl, wiall], [score])
                    else:
                        C.stt('dve', score[:, cols], rl[:, :w], wiall[:, nb, h:h + 1], score[:, cols], ALU.mult, ALU.add,
                              [rl, wiall, score], [score])
                C.ts('dve', qcj[:], qc[:, nb:nb + 1], -8.0 * j, None, ALU.add, None, [qc], [qcj])
                C.ts('dve', mbias[:, :w], kc512[:, :w], qcj[:, 0:1], MASKV, ALU.is_gt, ALU.mult, [kc512, qcj], [mbias])
                C.tt('pool', score[:, cols], score[:, cols], mbias[:, :w], ALU.add, [score, mbias], [score])
            C.memset('pool', bs[:, 0:1], MASKV, [bs])
            C.memset('pool', cnt[:], 0.0, [cnt])
            C.S.op('dve', lambda e: e.tensor_reduce(out=bs[:, 1:2], in_=score[:, :nk], axis=AX.X, op=ALU.max), [score], [bs])
            for it in range(NIT):
                C.tt('dve', bs[:, 2:3], bs[:, 0:1], bs[:, 1:2], ALU.add, [bs], [bs])
                C.ts('dve', bs[:, 2:3], bs[:, 2:3], 0.5, None, ALU.mult, None, [bs], [bs])
                C.ts('dve', Mb[:, :nk], score[:, :nk], bs[:, 2:3], 0.0, ALU.is_ge, ALU.add, [score, bs], [Mb, cnt],
                     accum_out=cnt[:, it:it + 1])
                C.ts('dve', bs[:, 3:4], cnt[:, it:it + 1], 255.5, None, ALU.is_ge, None, [cnt], [bs])
                C.tt('dve', bs[:, 4:5], bs[:, 2:3], bs[:, 0:1], ALU.subtract, [bs], [bs])
                C.tt('dve', bs[:, 5:6], bs[:, 1:2], bs[:, 2:3], ALU.subtract, [bs], [bs])
                C.stt('dve', bs[:, 0:1], bs[:, 4:5], bs[:, 3:4], bs[:, 0:1], ALU.mult, ALU.add, [bs], [bs])
                C.stt('dve', bs[:, 1:2], bs[:, 5:6], bs[:, 3:4], bs[:, 2:3], ALU.mult, ALU.add, [bs], [bs])
            C.ts('dve', bs[:, 6:7], bs[:, 0:1], MASKV + 1000.0, None, ALU.max, None, [bs], [bs])
            if dbg:
                C.ts('dve', Mb[:, :nk], score[:, :nk], bs[:, 6:7], 0.0, ALU.is_ge, ALU.add, [score, bs], [Mb, cdb],
                     accum_out=cdb[:, nb, 0:1])
                C.cp('dve', cdb[:, nb, 1:2], bs[:, 0:1], [bs], [cdb])
                C.cp('dve', cdb[:, nb, 2:3], bs[:, 1:2], [bs], [cdb])
                C.cp('dve', cdb[:, nb, 3:4], bs[:, 6:7], [bs], [cdb])
            else:
                C.ts('dve', Mb[:, :nk], score[:, :nk], bs[:, 6:7], None, ALU.is_ge, None, [score, bs], [Mb])
            for r_ in range(2):
                for i_ in range(8):
                    c = r_ * 8 + i_
                    C.tr(pTb[:, i_, :], sgt[:, c * 128:(c + 1) * 128], identb[:], [sgt, identb], [pTb])
                C.cp('act', sgT[:, r_ * 8:(r_ + 1) * 8, :], pTb[:], [pTb], [sgT])
            for hf in range(2):
                for kg in range((nkb + 7) // 8):
                    ng = min(8, nkb - kg * 8)
                    MT = C.ring('MT')
                    for i_ in range(ng):
                        kb = kg * 8 + i_
                        C.tr(pTb[:, i_, :], Mb[:, kb * 128:(kb + 1) * 128], identb[:], [Mb, identb], [pTb])
                    C.cp('act', MT[:, 0:ng, :], pTb[:, 0:ng, :], [pTb], [MT])
                    for i_ in range(ng):
                        kb = kg * 8 + i_
                        first, last = (kb == 0), (kb == nkb - 1)
                        for h2 in range(2):
                            hg = hf * 2 + h2
                            ps_ = C.ring('st')
                            C.mm(ps_[:], kTb[:, kb * 128:(kb + 1) * 128], QTs[:, hg * 4:(hg + 1) * 4, :].rearrange("p h q -> p (h q)"),
                                 True, True, [kTb, QTs], [ps_])
                            E = C.ring('E')
                            C.act(E[:].rearrange("p h q -> p (h q)"), ps_[:], AF.Exp, [ps_], [E], scale=SCALE)
                            P = C.ring('P')
                            C.tt('dve' if h2 == 0 else 'pool', P[:], E[:], bc_mid(MT[:, i_, :], 4), ALU.mult, [E, MT], [P])
                            C.mm(ACC[h2][:], Vb[:, kb, :], P[:].rearrange("p h q -> p (h q)"), first, last, [P, Vb], [ACC[h2]])
                            C.mm(RS[h2][:], onesb[:, :], P[:].rearrange("p h q -> p (h q)"), first, last, [P, onesb], [RS[h2]])
                for h2 in range(2):
                    hg = hf * 2 + h2
                    C.cp('act', tmpo[:], RS[h2][:], [RS[h2]], [tmpo])
                    C.S.op('dve', lambda e: e.reciprocal(out=tmpo[:], in_=tmpo[:]), [tmpo], [tmpo])
                    C.tt('dve', tmpo[:], ACC[h2][:], tmpo[:], ALU.mult, [ACC[h2], tmpo], [tmpo])
                    if dbg:
                        C.dma(odbg_d.ap()[nb, :, hg * 512:(hg + 1) * 512], tmpo[:], [tmpo], [odbg_d])
                    C.tt('pool', ogT[:, hg * 4:(hg + 1) * 4, :].rearrange("p h q -> p (h q)"), tmpo[:],
                         sgT[:, hg * 4:(hg + 1) * 4, :].rearrange("p h q -> p (h q)"), ALU.mult, [tmpo, sgT], [ogT])
            for half in range(2):
                po = C.ring('st')
                for c in range(16):
                    C.mm(po[:], ogT[:, c, :], Wo[:, c, half * 512:(half + 1) * 512], c == 0, c == 15, [ogT, Wo], [po])
                C.tt('dve', tmp32[:, half * 512:(half + 1) * 512], po[:], gt1[:, half * 512:(half + 1) * 512], ALU.mult, [po, gt1], [tmp32])
            C.tt('pool', x2[:], tmp32[:], xt[:], ALU.add, [tmp32, xt], [x2])
            C.memset('pool', ss[:, 0:1], 0.0, [ss])
            C.act(tmp32[:], x2[:], AF.Square, [x2, ss], [tmp32, ss], accum_out=ss[:, 0:1])
            C.ts('dve', ss[:, 1:2], ss[:, 0:1], 1.0 / 1024, 1e-6, ALU.mult, ALU.add, [ss], [ss])
            C.act(ss[:, 2:3], ss[:, 1:2], AF.Sqrt, [ss], [ss])
            C.S.op('dve', lambda e: e.reciprocal(out=ss[:, 3:4], in_=ss[:, 2:3]), [ss], [ss])
            C.stt('dve', ot[:], x2[:], ss[:, 3:4], fg[:], ALU.mult, ALU.mult, [x2, ss, fg], [ot])
            C.dma(out_d.ap()[sgi, blk * 128:(blk + 1) * 128, :], ot[:], [ot], [out_d], q='pool')
    if dbg:
        C.dma(cdbg_d.ap()[:, :, :], cdb[:], [cdb], [cdbg_d])
    C.S.finish('sp')
    C.es.close()
    return C.nc


def inputs_C2(inp, c1, x1, kT, v, kiT, b, hh, S):
    QS = S // 4
    NBS = QS // 128
    qs = QUARTERS[hh]
    x1s = np.stack([x1[b, q * QS:(q + 1) * QS] for q in qs])
    qcv = np.zeros((128, 2 * NBS), np.float32)
    for i, q in enumerate(qs):
        for blk in range(NBS):
            qcv[:, i * NBS + blk] = (q * QS + blk * 128 + np.arange(128)) // 64
    kc512 = rep((np.arange(512) // 64).astype(np.float32))
    cst = np.zeros((128, 256), np.float32)
    cst[:, 0:128] = np.eye(128)
    return {"QT": c1["QT"], "QiT": c1["QiT"], "sg": c1["sg"], "wi": c1["wi"], "x1": np.ascontiguousarray(x1s),
            "kT": np.ascontiguousarray(kT[b]), "kiT": np.ascontiguousarray(kiT[b]), "v": np.ascontiguousarray(v[b]),
            "qc": qcv, "kc512": kc512, "Wo1": np.ascontiguousarray(inp["odd_w_out"][0]), "cT": pcol(inp["c"][b]),
            "adaw1": np.ascontiguousarray(inp["ada_w"][1]), "adab1": rep(inp["ada_b"][1]), "fg_rep": rep(inp["final_g"]),
            "cst": cst}


def run_C2(inp, c1res, x1, kT, v, kiT, S, dbg=False):
    nc = build_C2(S, dbg)
    maps = [inputs_C2(inp, c1res[c], x1, kT, v, kiT, c // 2, c % 2, S) for c in range(NCORES)]
    res = run_bass_kernel_spmd(nc, maps, core_ids=list(range(NCORES)))
    QS = S // 4
    out = np.zeros((4, S, 1024), np.float32)
    for c in range(NCORES):
        b, hh = c // 2, c % 2
        for i, q in enumerate(QUARTERS[hh]):
            out[b, q * QS:(q + 1) * QS] = res.results[c]["out"][i]
    if dbg:
        return out, res.results
    return out


def kernel(**inputs):
    inp = {k: np.asarray(v) for k, v in inputs.items()}
    S = S_FULL
    yb = run_A(inp, S)
    x1, kT, v, kiT = run_B(inp, yb, S)
    c1 = run_C1(inp, x1, S)
    out = run_C2(inp, c1, x1, kT, v, kiT, S)
    return np.ascontiguousarray(out.astype(np.float32))
```
